# Optimizing a Trainium2 kernel written in Bass

```python
import jax, jax.numpy as jnp
from jax import lax
import numpy as np

D_MODEL = 1024
BATCH = 2
SEQ = 8192
DEPTH = 2

MIX_WIDTH = D_MODEL
ATTN_WIDTH = MIX_WIDTH // 2
HGRN_WIDTH = MIX_WIDTH - ATTN_WIDTH
HEAD_DIM = 64
N_ATTN_HEADS = ATTN_WIDTH // HEAD_DIM
HGRN_EXPAND = 64
N_HGRN_HEADS = HGRN_WIDTH // HGRN_EXPAND
HGRN_VDIM = HGRN_WIDTH // N_HGRN_HEADS
DILATED_PATTERNS = ((128, 1), (512, 4), (2048, 16))
ATTN_BLOCK = 128
HGRN_CHUNK = 64
ROPE_THETA = 10000.0
N_EXPERTS = 16
N_GROUPS = 4
EXPERTS_PER_GROUP = N_EXPERTS // N_GROUPS
TOP_K = 2
D_FF_EXPERT = 512
RMS_EPS = 1e-6
IN_COLS = 3 * ATTN_WIDTH + 4 * HGRN_WIDTH

kernel_name = 'hybrid_dilated_attn_hgrn2_grouped_moe_adaln'


def rms_norm(x, gain=None):
    xf = x.astype(jnp.float32)
    y = xf * lax.rsqrt(jnp.mean(xf * xf, axis=-1, keepdims=True) + RMS_EPS)
    if gain is not None:
        y = y * gain.astype(jnp.float32)
    return y


def rotary(t, positions):
    half = HEAD_DIM // 2
    inv_freq = ROPE_THETA ** (-jnp.arange(half, dtype=jnp.float32) / half)
    ang = positions.astype(jnp.float32)[:, None, :, None] * inv_freq
    cos, sin = jnp.cos(ang), jnp.sin(ang)
    t1, t2 = t[..., :half], t[..., half:]
    return jnp.concatenate([t1 * cos - t2 * sin, t1 * sin + t2 * cos], axis=-1)


def dilated_window_pattern(q, k, v, window, dilation):
    B, H, S, Dh = q.shape
    steps = window // dilation
    span = dilation * ATTN_BLOCK
    s_pad = -(-S // span) * span
    m = s_pad // dilation
    nb = m // ATTN_BLOCK

    def to_streams(t):
        t = jnp.pad(t, ((0, 0), (0, 0), (0, s_pad - S), (0, 0)))
        t = t.reshape(B, H, m, dilation, Dh).transpose(0, 1, 3, 2, 4)
        return t.reshape(B, H, dilation, nb, ATTN_BLOCK, Dh)

    qs, ks, vs = to_streams(q), to_streams(k), to_streams(v)

    def with_prev(t):
        prev = jnp.pad(t, ((0, 0), (0, 0), (0, 0), (1, 0), (0, 0), (0, 0)))[:, :, :, :-1]
        return jnp.concatenate([prev, t], axis=-2)

    kk, vv = with_prev(ks), with_prev(vs)
    scores = jnp.einsum('bhrnqd,bhrnkd->bhrnqk', qs, kk) * (HEAD_DIM ** -0.5)
    qi = jnp.arange(ATTN_BLOCK)[:, None]
    kj = jnp.arange(2 * ATTN_BLOCK)[None, :]
    dist = qi + ATTN_BLOCK - kj
    in_band = (dist >= 0) & (dist <= steps)
    first_block = (jnp.arange(nb) == 0)[:, None, None]
    valid = in_band[None] & ~(first_block & (kj < ATTN_BLOCK)[None])
    scores = jnp.where(valid, scores, -jnp.inf)
    mx = jnp.max(scores, axis=-1, keepdims=True)
    p = jnp.exp(scores - mx)
    den = jnp.sum(p, axis=-1, keepdims=True)
    out = jnp.einsum('bhrnqk,bhrnkd->bhrnqd', p, vv) / den

    def from_streams(t):
        X = t.shape[-1]
        t = t.reshape(B, H, dilation, m, X).transpose(0, 1, 3, 2, 4).reshape(B, H, s_pad, X)
        return t[:, :, :S]

    return from_streams(out), from_streams(mx), from_streams(den)


def dilated_mixture_attention(q, k, v):
    results = [dilated_window_pattern(q, k, v, w, d) for (w, d) in DILATED_PATTERNS]
    out = jnp.stack([r[0] for r in results])
    mx = jnp.stack([r[1] for r in results])
    den = jnp.stack([r[2] for r in results])
    weight = den * jnp.exp(mx - jnp.max(mx, axis=0, keepdims=True))
    return jnp.sum(weight * out, axis=0) / jnp.sum(weight, axis=0)


def hgrn2_mixer(q, f_raw, i, lower_bound):
    B, S, _ = q.shape
    C = HGRN_CHUNK
    nc = S // C
    q = jax.nn.silu(q)
    f = lower_bound + (1.0 - lower_bound) * jax.nn.sigmoid(f_raw)
    log_f = jnp.log(f)
    key = 1.0 - f

    def to_chunks(t, dim):
        return t.reshape(B, nc, C, N_HGRN_HEADS, dim).transpose(1, 0, 3, 2, 4)

    qc = to_chunks(q, HGRN_EXPAND)
    kc = to_chunks(key, HGRN_EXPAND)
    gc = to_chunks(log_f, HGRN_EXPAND)
    vc = to_chunks(i, HGRN_VDIM)
    causal = jnp.tril(jnp.ones((C, C), dtype=bool))[:, :, None]

    def chunk_step(state, inp):
        q_, k_, g_, v_ = inp
        b = jnp.cumsum(g_, axis=-2)
        o_inter = jnp.einsum('bhtk,bhkv->bhtv', q_ * jnp.exp(b), state)
        diff = b[:, :, :, None, :] - b[:, :, None, :, :]
        decay = jnp.where(causal, jnp.exp(jnp.where(causal, diff, 0.0)), 0.0)
        scores = jnp.einsum('bhtk,bhsk,bhtsk->bhts', q_, k_, decay)
        o_intra = jnp.einsum('bhts,bhsv->bhtv', scores, v_)
        b_last = b[:, :, -1:, :]
        new_state = jnp.exp(b_last)[:, :, 0, :, None] * state + jnp.einsum(
            'bhsk,bhsv->bhkv', k_ * jnp.exp(b_last - b), v_)
        return new_state, o_inter + o_intra

    state0 = jnp.zeros((B, N_HGRN_HEADS, HGRN_EXPAND, HGRN_VDIM), jnp.float32)
    _, o = lax.scan(chunk_step, state0, (qc, kc, gc, vc))
    return o.transpose(1, 0, 3, 2, 4).reshape(B, S, HGRN_WIDTH)


def mixing_sublayer(h, positions, w_in, w_out, attn_norm, hgrn_norm, lower_bound):
    B, S, _ = h.shape
    proj = jnp.einsum('bsd,de->bse', h, w_in).astype(jnp.float32)
    cuts = np.cumsum([ATTN_WIDTH] * 3 + [HGRN_WIDTH] * 3).tolist()
    q_a, k_a, v_a, q_h, f_h, i_h, g_h = jnp.split(proj, cuts, axis=-1)

    def heads(t):
        return t.reshape(B, S, N_ATTN_HEADS, HEAD_DIM).transpose(0, 2, 1, 3)

    q_a = rotary(heads(q_a), positions)
    k_a = rotary(heads(k_a), positions)
    attn = dilated_mixture_attention(q_a, k_a, heads(v_a))
    attn = attn.transpose(0, 2, 1, 3).reshape(B, S, ATTN_WIDTH)
    rec = hgrn2_mixer(q_h, f_h, i_h, lower_bound)
    rec = rms_norm(rec, hgrn_norm) * jax.nn.sigmoid(g_h)
    merged = jnp.concatenate([rms_norm(attn, attn_norm), rec], axis=-1)
    return jnp.einsum('bse,ed->bsd', merged.astype(w_out.dtype), w_out)


def grouped_moe(h, w_router, b_router, w_gate, w_up, w_down):
    B, S, D = h.shape
    T = B * S
    hf = h.reshape(T, D)
    logits = (hf @ w_router).astype(jnp.float32) + b_router.astype(jnp.float32)
    probs = jax.nn.softmax(logits, axis=-1)
    pg = probs.reshape(T, N_GROUPS, EXPERTS_PER_GROUP)
    top_in_group, _ = lax.top_k(pg, TOP_K)
    g_sel = jnp.argmax(jnp.sum(top_in_group, axis=-1), axis=-1)
    in_group = jnp.einsum('tg,tge->te', jax.nn.one_hot(g_sel, N_GROUPS, dtype=jnp.float32), pg)
    vals, idx = lax.top_k(in_group, TOP_K)
    wts = vals / jnp.sum(vals, axis=-1, keepdims=True)
    expert_idx = g_sel[:, None] * EXPERTS_PER_GROUP + idx
    combine = jnp.einsum('tke,tk->te', jax.nn.one_hot(expert_idx, N_EXPERTS, dtype=jnp.float32), wts)
    y = jnp.zeros((T, D), jnp.float32)
    for e in range(N_EXPERTS):
        a = jax.nn.silu(hf @ w_gate[e]) * (hf @ w_up[e])
        y = y + combine[:, e:e + 1] * (a @ w_down[e]).astype(jnp.float32)
    return y.reshape(B, S, D)


def setup_inputs(seed: int = 0) -> dict:
    key = jax.random.key(seed)
    ks = jax.random.split(key, 16)
    D, E, F = D_MODEL, N_EXPERTS, D_FF_EXPERT
    offsets = jax.random.randint(ks[2], (BATCH, 1), 0, 1024, dtype=jnp.int32)
    positions = (offsets + jnp.arange(SEQ, dtype=jnp.int32)[None, :]).astype(jnp.int32)
    return {
        'x': jax.random.normal(ks[0], (BATCH, SEQ, D), jnp.float32),
        'c': jax.random.normal(ks[1], (BATCH, D), jnp.float32),
        'positions': positions,
        'w_in': jax.random.normal(ks[3], (DEPTH, D, IN_COLS), jnp.float32) * D ** -0.5,
        'w_out': jax.random.normal(ks[4], (DEPTH, MIX_WIDTH, D), jnp.float32) * MIX_WIDTH ** -0.5,
        'attn_norm': 1.0 + 0.05 * jax.random.normal(ks[5], (DEPTH, ATTN_WIDTH), jnp.float32),
        'hgrn_norm': 1.0 + 0.05 * jax.random.normal(ks[6], (DEPTH, HGRN_WIDTH), jnp.float32),
        'lb_params': 0.5 * jax.random.normal(ks[7], (DEPTH, HGRN_WIDTH), jnp.float32),
        'ada_w': jax.random.normal(ks[8], (DEPTH, D, 6 * D), jnp.float32) * (0.5 * D ** -0.5),
        'ada_b': 0.02 * jax.random.normal(ks[9], (DEPTH, 6 * D), jnp.float32),
        'w_router': jax.random.normal(ks[10], (D, E), jnp.float32) * D ** -0.5,
        'b_router': 0.01 * jax.random.normal(ks[11], (E,), jnp.float32),
        'w_gate': jax.random.normal(ks[12], (DEPTH, E, D, F), jnp.float32) * D ** -0.5,
        'w_up': jax.random.normal(ks[13], (DEPTH, E, D, F), jnp.float32) * D ** -0.5,
        'w_down': jax.random.normal(ks[14], (DEPTH, E, F, D), jnp.float32) * F ** -0.5,
        'final_norm': 1.0 + 0.05 * jax.random.normal(ks[15], (D,), jnp.float32),
    }


def reference(x, c, positions, w_in, w_out, attn_norm, hgrn_norm, lb_params, ada_w, ada_b,
              w_router, b_router, w_gate, w_up, w_down, final_norm):
    p = jax.nn.softmax(lb_params.astype(jnp.float32), axis=0)
    lower_bounds = jnp.cumsum(p, axis=0) - p[0:1]
    c_act = jax.nn.silu(c)
    for l in range(DEPTH):
        mod = (jnp.einsum('bd,de->be', c_act, ada_w[l]) + ada_b[l]).astype(jnp.float32)
        sh1, sc1, g1, sh2, sc2, g2 = jnp.split(mod[:, None, :], 6, axis=-1)
        h = (rms_norm(x) * (1.0 + sc1) + sh1).astype(x.dtype)
        mix = mixing_sublayer(h, positions, w_in[l], w_out[l], attn_norm[l], hgrn_norm[l],
                              lower_bounds[l])
        x = x + (g1 * mix.astype(jnp.float32)).astype(x.dtype)
        h = (rms_norm(x) * (1.0 + sc2) + sh2).astype(x.dtype)
        ffn = grouped_moe(h, w_router, b_router, w_gate[l], w_up[l], w_down[l])
        x = x + (g2 * ffn).astype(x.dtype)
    return rms_norm(x, final_norm).astype(x.dtype)
```

```python
import numpy as np
import ml_dtypes
from contextlib import ExitStack
import concourse.bass as bass
import concourse.mybir as mybir
from concourse.bass_utils import run_bass_kernel_spmd

F32 = mybir.dt.float32
BF16 = mybir.dt.bfloat16
I32 = mybir.dt.int32
AF = mybir.ActivationFunctionType
ALU = mybir.AluOpType
AX = mybir.AxisListType

NCORES = 8
D = 1024
TOK = 2048
NTT = 4
NTB = 16
DEPTH = 2
INC = 3584
NE = 16
FF = 512
EPS = 1e-6
ENGS = ("pe", "act", "dve", "pool", "sp")


class Tl:
    def __init__(self, t, name=""):
        self.t = t
        self.name = name
        self.st = {}

    def __getitem__(self, key):
        return self.t[key]


class Prog:
    def __init__(self, nc, n_dma_sems=8):
        self.nc = nc
        self.ops = {e: [] for e in ENGS}
        self.cnt = {e: 0 for e in ENGS}
        self.sem = {}
        self.waited = {}
        self.dma_sems = {}
        self.dma_rr = {e: 0 for e in ENGS}
        self.n_dma_sems = n_dma_sems
        self.dma_val = {}
        self.final_events = []

    def open(self, stack):
        for e in ENGS:
            self.sem[e] = stack.enter_context(self.nc.semaphore("s_" + e))
        for e in ("sp", "pool", "act"):
            self.dma_sems[e] = [stack.enter_context(self.nc.semaphore(f"d_{e}{i}")) for i in range(self.n_dma_sems)]
            for i in range(self.n_dma_sems):
                self.dma_val[(e, i)] = 0

    def _st(self, tk):
        if isinstance(tk, tuple):
            t, k = tk
        else:
            t, k = tk, None
        return t.st.setdefault(k, {"w": None, "r": []})

    def _deps(self, reads, writes):
        deps = []
        for tk in reads:
            s = self._st(tk)
            if s["w"] is not None:
                deps.append(s["w"])
            t = tk[0] if isinstance(tk, tuple) else tk
            if getattr(t, "excl", False):
                deps.extend(s["r"])
        for tk in writes:
            s = self._st(tk)
            if s["w"] is not None:
                deps.append(s["w"])
            deps.extend(s["r"])
        return deps

    def _commit(self, ev, reads, writes):
        for tk in reads:
            s = self._st(tk)
            s["r"] = [r for r in s["r"] if r[0] != ev[0]] + [ev]
        for tk in writes:
            s = self._st(tk)
            s["w"] = ev
            s["r"] = []

    def _semh(self, sk):
        if sk in self.sem:
            return self.sem[sk]
        e, i = sk
        return self.dma_sems[e][i]

    def _emit_waits(self, eng, deps):
        need = {}
        for sk, val, owner in deps:
            if owner == eng and eng == "pe":
                continue
            if self.waited.get((eng, sk), 0) >= val:
                continue
            if need.get(sk, 0) < val:
                need[sk] = val
        for sk, val in need.items():
            self.waited[(eng, sk)] = val
            sem = self._semh(sk)
            self.ops[eng].append(lambda e, sem=sem, val=val: e.wait_ge(sem, val))

    def op(self, eng, fn, reads=(), writes=()):
        self._emit_waits(eng, self._deps(reads, writes))
        self.cnt[eng] += 1
        sem = self.sem[eng]
        self.ops[eng].append(lambda e, fn=fn, sem=sem: fn(e).then_inc(sem, 1))
        ev = (eng, self.cnt[eng], eng)
        self._commit(ev, reads, writes)
        return ev

    def dma(self, q, out, in_, reads=(), writes=(), final=False, **kw):
        deps = self._deps(reads, writes)
        i = self.dma_rr[q]
        self.dma_rr[q] = (i + 1) % self.n_dma_sems
        sk = (q, i)
        prev = self.dma_val[sk]
        if prev > 0:
            deps.append((sk, prev, None))
        self._emit_waits(q, deps)
        val = prev + 16
        self.dma_val[sk] = val
        sem = self.dma_sems[q][i]
        self.ops[q].append(lambda e, out=out, in_=in_, sem=sem, kw=kw: e.dma_start(out=out, in_=in_, **kw).then_inc(sem, 16))
        ev = (sk, val, None)
        self._commit(ev, reads, writes)
        if final:
            self.final_events.append(ev)
        return ev

    def barrier(self):
        for eng in ENGS:
            deps = [(e2, self.cnt[e2], e2) for e2 in ENGS if e2 != eng and self.cnt[e2] > 0]
            deps += [(sk, v, None) for sk, v in self.dma_val.items() if v > 0]
            self._emit_waits(eng, deps)

    def finish(self):
        self._emit_waits("sp", self.final_events)
        with self.nc.allow_low_precision("bf16 matmul operands, fp32 accumulation"), \
                self.nc.allow_non_contiguous_dma("strided layouts"), self.nc.Block() as block:
            @block.tensor
            def _(e):
                for f in self.ops["pe"]:
                    f(e)

            @block.scalar
            def _(e):
                for f in self.ops["act"]:
                    f(e)

            @block.vector
            def _(e):
                for f in self.ops["dve"]:
                    f(e)

            @block.gpsimd
            def _(e):
                for f in self.ops["pool"]:
                    f(e)

            @block.sync
            def _(e):
                for f in self.ops["sp"]:
                    f(e)


class Ring:
    def __init__(self, tiles):
        self.tiles = tiles
        self.i = 0

    def next(self):
        t = self.tiles[self.i]
        self.i = (self.i + 1) % len(self.tiles)
        return t


CO = {}


def _layout():
    off = 0
    for name, w in (("ident", 128), ("cum", 260), ("ut32", 128), ("perm", 128), ("maska", 256),
                    ("invf", 1), ("sign", 1), ("seln0", 128), ("seln1", 128), ("seld0", 128), ("seld1", 128),
                    ("ones", 128), ("ind", 4)):
        CO[name] = (off, w)
        off += w
    return off


NCONST = _layout()


def make_consts():
    c = np.zeros((128, NCONST), np.float32)

    def put(name, arr):
        o, w = CO[name]
        c[:arr.shape[0], o:o + w] = arr

    s = np.arange(128)[:, None]
    t = np.arange(128)[None, :]
    same = (s // 32) == (t // 32)
    put("ident", np.eye(128))
    ind = (s // 32 == np.arange(4)[None, :]).astype(np.float32)
    put("cum", np.concatenate([(same & (s <= t)), (s <= t), ind], axis=1).astype(np.float32))
    put("ind", ind)
    put("ut32", (same & (s > t)).astype(np.float32))
    m = np.arange(128)
    sw = (m // 64) * 64 + ((m % 64) + 32) % 64
    perm = np.zeros((128, 128), np.float32)
    perm[sw, m] = 1.0
    put("perm", perm)
    put("maska", np.concatenate([(s >= t), (s <= t)], axis=1).astype(np.float32))
    p = np.arange(128)
    invf = (10000.0 ** (-(p % 32).astype(np.float64) / 32.0)) / (2 * np.pi)
    put("invf", invf[:, None].astype(np.float32))
    put("sign", np.where((p % 64) < 32, -1.0, 1.0)[:, None].astype(np.float32))
    i = np.arange(64)
    for nm, rows, cols in (("seln0", i, i), ("seln1", i, 64 + i), ("seld0", 64 + i, i), ("seld1", 64 + i, 64 + i)):
        a = np.zeros((128, 128), np.float32)
        a[rows, cols] = 1.0
        put(nm, a)
    put("ones", np.ones((128, 128), np.float32))
    return c


class K:
    def __init__(self, stages):
        self.stages = stages
        self.nc = bass.Bass("TRN2", target_bir_lowering=False)
        self.st = ExitStack()
        self.P = Prog(self.nc)
        self.dram = {}
        self.scopes = [self.st]

    def scope(self):
        from contextlib import contextmanager

        @contextmanager
        def cm():
            es = ExitStack()
            self.scopes.append(es)
            try:
                yield
            finally:
                self.P.barrier()
                self.scopes.pop()
                es.close()
        return cm()

    def sb(self, shape, dt, name):
        self.uid = getattr(self, "uid", 0) + 1
        import os
        if os.environ.get("KDEBUG_ALLOC"):
            nb = int(np.prod(shape[1:])) * (4 if dt in (F32, I32) else 2)
            print(f"ALLOC depth={len(self.scopes)} {name} {nb}")
        return Tl(self.scopes[-1].enter_context(self.nc.sbuf_tensor(f"{name}_{self.uid}", list(shape), dt)), name)

    def ring(self, n, shape, dt, name):
        return Ring([self.sb(shape, dt, f"{name}{i}") for i in range(n)])

    def din(self, name, shape, dt):
        t = Tl(self.nc.dram_tensor(name, list(shape), dt, kind="ExternalInput").ap(), name)
        self.dram[name] = t
        return t

    def dout(self, name, shape, dt):
        t = Tl(self.nc.dram_tensor(name, list(shape), dt, kind="ExternalOutput").ap(), name)
        self.dram[name] = t
        return t

    def MM(self, out, lhsT, rhs, start=True, stop=True, R=(), W=()):
        return self.P.op("pe", lambda e: e.matmul(out, lhsT=lhsT, rhs=rhs, start=start, stop=stop), R, W)

    def ACT(self, out, in_, func, R=(), W=(), bias=None, scale=None):
        kw = {}
        if bias is not None:
            kw["bias"] = bias
        if scale is not None:
            kw["scale"] = scale
        return self.P.op("act", lambda e: e.activation(out, in_, func, **kw), R, W)

    def TT(self, eng, out, in0, in1, op, R=(), W=()):
        return self.P.op(eng, lambda e: e.tensor_tensor(out, in0, in1, op), R, W)

    def TS(self, eng, out, in0, s1, s2, op0, op1=None, R=(), W=()):
        if op1 is None:
            return self.P.op(eng, lambda e: e.tensor_scalar(out, in0, s1, None, op0), R, W)
        return self.P.op(eng, lambda e: e.tensor_scalar(out, in0, s1, s2, op0, op1), R, W)

    def STT(self, eng, out, in0, scalar, in1, op0, op1, R=(), W=()):
        return self.P.op(eng, lambda e: e.scalar_tensor_tensor(out, in0, scalar, in1, op0, op1), R, W)

    def CP(self, eng, out, in_, R=(), W=()):
        if eng == "act":
            return self.P.op("act", lambda e: e.copy(out, in_), R, W)
        return self.P.op(eng, lambda e: e.tensor_copy(out, in_), R, W)

    def MS(self, eng, ap, val, W=()):
        return self.P.op(eng, lambda e: e.memset(ap, val), (), W)

    def RECIP(self, out, in_, R=(), W=()):
        return self.P.op("dve", lambda e: e.reciprocal(out, in_), R, W)

    def psum(self):
        return self.PS.next()

    def setup(self, layers):
        nc, P = self.nc, self.P
        P.open(self.st)
        self.PS = Ring([Tl(self.st.enter_context(nc.psum_tensor(f"ps{i}", [128, 512], F32)), f"ps{i}") for i in range(8)])
        for t in self.PS.tiles:
            t.excl = True
        cst = self.din("consts", [128, NCONST], F32)
        self.CF = self.sb([128, NCONST], F32, "constf")
        P.dma("sp", self.CF[:], cst.t, writes=[self.CF])
        self.CB = self.sb([128, NCONST], BF16, "constb")
        self.CP("dve", self.CB[:], self.CF[:], R=[self.CF], W=[self.CB])
        pc = self.din("percore", [128, 16], F32)
        self.PC = self.sb([128, 16], F32, "percore_s")
        P.dma("sp", self.PC[:], pc.t, writes=[self.PC])
        self.epsc = self.sb([128, 1], F32, "epsc")
        self.MS("dve", self.epsc[:], EPS, W=[self.epsc])
        self.negpi = self.sb([128, 1], F32, "negpi")
        self.MS("dve", self.negpi[:], float(-np.pi), W=[self.negpi])
        self.F512 = self.ring(4, [128, 512], F32, "f512_")
        self.B512 = self.ring(4, [128, 512], BF16, "b512_")
        self.RSTD = self.ring(2, [128, 512], F32, "rstd_")
        self.XT = self.sb([128, 8, TOK], F32, "XT")
        self.HT = self.sb([128, 8, TOK], BF16, "HT")
        self.MOD = self.sb([128, DEPTH, 48], F32, "MOD")
        self.SCP = self.sb([128, DEPTH, 48], F32, "SCP")
        with self.scope():
            self._mods(layers)

    def cf(self, name, rows=128):
        o, w = CO[name]
        return self.CF[0:rows, o:o + w]

    def cb(self, name, rows=128):
        o, w = CO[name]
        return self.CB[0:rows, o:o + w]

    def _mods(self, layers):
        P = self.P
        cT = self.din("cT", [128, 8], F32)
        adaw = self.din("ada_w", [DEPTH, D, 6 * D], F32)
        adab = self.din("ada_bT", [128, DEPTH, 48], F32)
        c0 = self.sb([128, 8], F32, "c0")
        P.dma("sp", c0[:], cT.t, writes=[c0])
        ce = self.sb([128, 8], F32, "ce")
        self.ACT(ce[:], c0[:], AF.Exp, R=[c0], W=[ce], scale=-1.0)
        self.TS("dve", ce[:], ce[:], 1.0, None, ALU.add, R=[ce], W=[ce])
        self.RECIP(ce[:], ce[:], R=[ce], W=[ce])
        cact = self.sb([128, 8], F32, "cact")
        self.TT("dve", cact[:], c0[:], ce[:], ALU.mult, R=[c0, ce], W=[cact])
        bT = self.sb([128, DEPTH, 48], F32, "adabT")
        P.dma("sp", bT[:], adab.t, writes=[bT])
        aw = self.ring(2, [128, 8, 512], F32, "adaw")
        for l in layers:
            ps = self.psum()
            for pc in range(12):
                w = aw.next()
                P.dma("sp", w[:], adaw.t[l, :, pc * 512:(pc + 1) * 512].rearrange("(dc p) c -> p dc c", p=128), writes=[w])
                for cc in range(4):
                    col = pc * 4 + cc
                    for dc in range(8):
                        self.MM(ps[:, col:col + 1], w[:, dc, cc * 128:(cc + 1) * 128], cact[:, dc:dc + 1],
                                start=(dc == 0), stop=(dc == 7), R=[w, cact], W=[ps])
            self.TT("dve", self.MOD[:, l, :], ps[:, 0:48], bT[:, l, :], ALU.add, R=[ps, bT], W=[self.MOD])
        self.TS("dve", self.SCP[:], self.MOD[:], 1.0, None, ALU.add, R=[self.MOD], W=[self.SCP])
        if "dbgs" in self.dram:
            dd = self.dram["dbgs"]
            P.dma("sp", dd.t[:, 0:8], c0[:], reads=[c0], final=True)
            P.dma("sp", dd.t[:, 8:16], ce[:], reads=[ce], final=True)
            P.dma("sp", dd.t[:, 16:24], cact[:], reads=[cact], final=True)
            P.dma("sp", dd.t[:, 24:120], bT[:].rearrange("p l c -> p (l c)"), reads=[bT], final=True)

    def mod(self, l, j, dc, plus1=False):
        t = self.SCP if plus1 else self.MOD
        return t[:, l, j * 8 + dc:j * 8 + dc + 1]

    def load_x_tokmajor(self, xin):
        P = self.P
        self.scopes.append(ExitStack())
        xr = self.ring(2, [128, D], F32, "xtok")
        idf = self.cf("ident")
        for tb in range(NTB):
            xt = xr.next()
            P.dma("sp", xt[:], xin.t[tb * 128:(tb + 1) * 128, :], writes=[xt])
            for hf in range(2):
                ps = self.psum()
                for j in range(4):
                    dc = hf * 4 + j
                    self.MM(ps[:, j * 128:(j + 1) * 128], xt[:, dc * 128:(dc + 1) * 128], idf, R=[xt, self.CF], W=[ps])
                dst = self.XT[:, hf * 4:(hf + 1) * 4, tb * 128:(tb + 1) * 128]
                src = ps[:, :].rearrange("p (j t) -> p j t", j=4)
                self.CP("act" if hf == 0 else "dve", dst, src, R=[ps], W=[(self.XT, tb // 4)])
        self.P.barrier()
        self.scopes.pop().close()

    def load_xT(self, xTin):
        for dc in range(8):
            self.P.dma("sp", self.XT[:, dc, :], xTin.t[dc * 128:(dc + 1) * 128, :], writes=[(self.XT, tt) for tt in range(NTT)])

    def store_xT(self, xTout, final=True):
        for dc in range(8):
            self.P.dma("sp", xTout.t[dc * 128:(dc + 1) * 128, :], self.XT[:, dc, :], reads=[(self.XT, tt) for tt in range(NTT)],
                       writes=[xTout], final=final)

    def rms_stats(self, src_fn, nch, n_feat, tt_reads):
        ps = self.psum()
        for ch in range(nch):
            sq = self.B512.next()
            self.ACT(sq[:], src_fn(ch), AF.Square, R=tt_reads, W=[sq])
            self.MM(ps[:], self.cb("ones"), sq[:], start=(ch == 0), stop=(ch == nch - 1), R=[sq, self.CB], W=[ps])
        lnv = self.F512.next()
        self.ACT(lnv[:], ps[:], AF.Ln, R=[ps, self.epsc], W=[lnv], bias=self.epsc[:], scale=1.0 / n_feat)
        rstd = self.RSTD.next()
        self.ACT(rstd[:], lnv[:], AF.Exp, R=[lnv], W=[rstd], scale=-0.5)
        return rstd

    def norm_mod(self, l, which):
        jsh, jsc = (0, 1) if which == 0 else (3, 4)
        for tt in range(NTT):
            sl = slice(tt * 512, (tt + 1) * 512)
            rstd = self.rms_stats(lambda dc: self.XT[:, dc, sl], 8, D, [(self.XT, tt)])
            for dc in range(8):
                tmp = self.F512.next()
                self.TT("dve", tmp[:], self.XT[:, dc, sl], rstd[:], ALU.mult, R=[(self.XT, tt), rstd], W=[tmp])
                self.ACT(self.HT[:, dc, sl], tmp[:], AF.Identity, R=[tmp, self.SCP, self.MOD], W=[(self.HT, tt)],
                         bias=self.mod(l, jsh, dc), scale=self.mod(l, jsc, dc, plus1=True))
                if which == 1:
                    self.norm2_hook(l, tt, dc, tmp)
                if getattr(self, "dbg_norm", None) is not None and tt == 0 and dc == 0:
                    self.P.dma("sp", self.dbg_norm.t[0:128, 0:512], rstd[:], reads=[rstd], final=True)
                    self.P.dma("sp", self.dbg_norm.t[128:256, 0:512], tmp[:], reads=[tmp], final=True)

    def norm2_hook(self, l, tt, dc, tmp):
        pass

    def load_wg(self, w_in, l, g):
        w = self.WG.next()
        for dc in range(8):
            self.P.dma("pool", w[:, dc, :], w_in.t[l, dc * 128:(dc + 1) * 128, g * 512:(g + 1) * 512], writes=[w])
        return w

    def stage_A(self, l, io):
        P = self.P
        w_in = self.dram["w_in"]
        self.norm_mod(l, 0)
        with self.scope():
            self.WG = self.ring(4, [128, 8, 512], BF16, "wg")
            with self.scope():
                self._hgrn_local(l, io, w_in)
            with self.scope():
                self._attn_proj(l, io, w_in)

    def _lb_tiles(self, l):
        lb = self.sb([128, 512], F32, f"lb{l}")
        oml = self.sb([128, 512], F32, f"oml{l}")
        if "lb_params" not in self.dram:
            self.din("lb_params", [DEPTH, 512], F32)
        lbp = self.dram["lb_params"]
        if l == 0:
            self.MS("dve", lb[:], 0.0, W=[lb])
        else:
            with self.scope():
                raw = self.sb([128, DEPTH, 512], F32, "lbraw")
                for i in range(DEPTH):
                    self.P.dma("sp", raw[:, i, :], lbp.t[i:i + 1, :].partition_broadcast(128), writes=[raw])
                mx = self.sb([128, 512], F32, "lbmx")
                self.TT("dve", mx[:], raw[:, 0, :], raw[:, 1, :], ALU.max, R=[raw], W=[mx])
                for i in range(DEPTH):
                    self.TT("dve", raw[:, i, :], raw[:, i, :], mx[:], ALU.subtract, R=[raw, mx], W=[raw])
                    self.ACT(raw[:, i, :], raw[:, i, :], AF.Exp, R=[raw], W=[raw])
                self.TT("dve", mx[:], raw[:, 0, :], raw[:, 1, :], ALU.add, R=[raw], W=[mx])
                self.RECIP(mx[:], mx[:], R=[mx], W=[mx])
                self.TT("dve", lb[:], raw[:, 1, :], mx[:], ALU.mult, R=[raw, mx], W=[lb])
        self.TS("dve", oml[:], lb[:], -1.0, 1.0, ALU.mult, ALU.add, R=[lb], W=[oml])
        return lb, oml

    def _proj_fm(self, w, pair, tt):
        ps = self.psum()
        sl = slice(tt * 512, (tt + 1) * 512)
        for dc in range(8):
            self.MM(ps[:], w[:, dc, pair * 128:(pair + 1) * 128], self.HT[:, dc, sl], start=(dc == 0), stop=(dc == 7),
                    R=[w, (self.HT, tt)], W=[ps])
        return ps

    def _proj_tm(self, w, tb):
        ps = self.psum()
        for dc in range(8):
            self.MM(ps[:], self.HT[:, dc, tb * 128:(tb + 1) * 128], w[:, dc, :], start=(dc == 0), stop=(dc == 7),
                    R=[w, (self.HT, tb // 4)], W=[ps])
        return ps

    def _hgrn_local(self, l, io, w_in):
        P = self.P
        lb, oml = self._lb_tiles(l)
        wq = self.load_wg(w_in, l, 3)
        wf = self.load_wg(w_in, l, 4)
        wi = self.load_wg(w_in, l, 5)
        wg = self.load_wg(w_in, l, 6)
        if True:
            self.QS = self.ring(1, [128, 4, 512], BF16, "QS")
            self.GTt = self.ring(1, [128, 4, 512], BF16, "GTt")
            self.QHt = self.ring(1, [128, 4, 512], BF16, "QHt")
            self.OTt = self.ring(1, [128, 4, 512], F32, "OTt")
            self.Gtm = self.ring(2, [128, 512], F32, "Gtm")
            self.KKtm = self.ring(2, [128, 512], BF16, "KKtm")
            self.KDtm = self.ring(2, [128, 512], BF16, "KDtm")
            self.Vtm = self.ring(2, [128, 512], BF16, "Vtm")
            self.VBLK = self.ring(1, [128, 8, 4, 64], BF16, "VBLK")
            self.S32 = [self.sb([128, 64], F32, f"S32_{p}") for p in range(4)]
            self.SBF = [self.ring(5, [128, 128], BF16, f"SBF{p}_") for p in range(4)]
            self.QTZ = self.ring(3, [128, 2, 128], BF16, "QTZ")
            for p in range(4):
                for t in self.SBF[p].tiles:
                    self.MS("dve", t[:], 0.0, W=[t])
            for t in self.QTZ.tiles:
                self.MS("dve", t[:], 0.0, W=[t])
            self.BBLK = self.sb([128, 4], F32, "BBLK")
            self.E128 = self.ring(6, [128, 128], F32, "E128_")
            self.B128 = self.ring(8, [128, 128], BF16, "B128_")
            self.SM = self.ring(3, [128, 256], BF16, "SMh")
            self.ED = self.ring(4, [128, 4], F32, "EDh")
            self.DOUT = self.sb([128, 4], F32, "DOUT")
        for p in range(4):
            self.MS("dve", self.S32[p][:], 0.0, W=[self.S32[p]])
        self.MS("dve", self.BBLK[:], 0.0, W=[self.BBLK])
        scur = []
        for p in range(4):
            s0 = self.SBF[p].next()
            for hh in range(2):
                r = slice(hh * 64, (hh + 1) * 64)
                self.MS("dve", s0[r, r], 0.0, W=[s0])
            scur.append(s0)
        cum = self.cf("cum")
        import os
        SK = os.environ.get("KSKIP", "")
        for tt in range(1 if "1" in SK else NTT):
            sl = slice(tt * 512, (tt + 1) * 512)
            QS = self.QS.next()
            GT = self.GTt.next()
            QH = self.QHt.next()
            OT = self.OTt.next()
            for pair in range(4):
                ps = self._proj_fm(wq, pair, tt)
                e = self.F512.next()
                self.ACT(e[:], ps[:], AF.Exp, R=[ps], W=[e], scale=-1.0)
                self.TS("dve", e[:], e[:], 1.0, None, ALU.add, R=[e], W=[e])
                self.RECIP(e[:], e[:], R=[e], W=[e])
                self.TT("dve", QS[:, pair, :], ps[:], e[:], ALU.mult, R=[ps, e], W=[QS])
                ps = self._proj_fm(wg, pair, tt)
                e = self.F512.next()
                self.ACT(e[:], ps[:], AF.Exp, R=[ps], W=[e], scale=-1.0)
                self.TS("dve", e[:], e[:], 1.0, None, ALU.add, R=[e], W=[e])
                self.P.op("dve", lambda en, o=GT[:, pair, :], i=e[:]: en.reciprocal(o, i), [e], [GT])
            for tbl in range(4):
                if "T" in SK:
                    continue
                tb = tt * 4 + tbl
                bsl = slice(tbl * 128, (tbl + 1) * 128)
                ps = self._proj_tm(wf, tb)
                e = self.F512.next()
                self.ACT(e[:], ps[:], AF.Exp, R=[ps], W=[e], scale=-1.0)
                self.TS("dve", e[:], e[:], 1.0, None, ALU.add, R=[e], W=[e])
                self.RECIP(e[:], e[:], R=[e], W=[e])
                self.TT("dve", e[:], e[:], oml[:], ALU.mult, R=[e, oml], W=[e])
                self.TT("dve", e[:], e[:], lb[:], ALU.add, R=[e, lb], W=[e])
                G = self.Gtm.next()
                self.ACT(G[:], e[:], AF.Ln, R=[e], W=[G])
                KK = self.KKtm.next()
                self.TS("dve", KK[:], e[:], -1.0, 1.0, ALU.mult, ALU.add, R=[e], W=[KK])
                psd = self.psum()
                self.MM(psd[:], self.cf("ut32"), G[:], R=[G, self.CF], W=[psd])
                edl = self.F512.next()
                self.ACT(edl[:], psd[:], AF.Exp, R=[psd], W=[edl])
                KD = self.KDtm.next()
                self.TT("dve", KD[:], KK[:], edl[:], ALU.mult, R=[KK, edl], W=[KD])
                psv = self._proj_tm(wi, tb)
                V = self.Vtm.next()
                self.CP("act", V[:], psv[:], R=[psv], W=[V])
                VB = self.VBLK.next()
                vsrc = V[:, :].rearrange("p (h c) -> p h c", h=8).unsqueeze(2).to_broadcast([128, 8, 4, 64])
                isrc = self.cb("ind").unsqueeze(1).unsqueeze(3).to_broadcast([128, 8, 4, 64])
                if "B" not in SK:
                    self.TT("dve", VB[:], vsrc, isrc, ALU.mult, R=[V, self.CB], W=[VB])
                for pair in range(4):
                    if "P" in SK:
                        continue
                    psl = slice(pair * 128, (pair + 1) * 128)
                    pb = self.psum()
                    self.MM(pb[:, 0:260], G[:, psl], cum, R=[G, self.CF], W=[pb])
                    self.MM(pb[:, 264:392], KK[:, psl], self.cb("ident"), R=[KK, self.CB], W=[pb])
                    Eb = self.E128.next()
                    self.ACT(Eb[:], pb[:, 0:128], AF.Exp, R=[pb], W=[Eb])
                    Enb = self.E128.next()
                    self.ACT(Enb[:], pb[:, 0:128], AF.Exp, R=[pb], W=[Enb], scale=-1.0)
                    EB = self.E128.next()
                    self.ACT(EB[:], pb[:, 128:256], AF.Exp, R=[pb, self.BBLK], W=[EB], bias=self.BBLK[:, pair:pair + 1])
                    ed = self.ED.next()
                    self.ACT(ed[:], pb[:, 256:260], AF.Exp, R=[pb], W=[ed])
                    qt = self.B128.next()
                    self.TT("dve", qt[:], QS[:, pair, bsl], Eb[:], ALU.mult, R=[QS, Eb], W=[qt])
                    qz = self.QTZ.next()
                    for hh in range(2):
                        r = slice(hh * 64, (hh + 1) * 64)
                        self.TT("dve", qz[r, hh, :], QS[r, pair, bsl], Eb[r, :], ALU.mult, R=[QS, Eb], W=[qz])
                    kt = self.B128.next()
                    self.TT("dve", kt[:], pb[:, 264:392], Enb[:], ALU.mult, R=[pb, Enb], W=[kt])
                    self.TT("dve", QH[:, pair, bsl], QS[:, pair, bsl], EB[:], ALU.mult, R=[QS, EB], W=[QH])
                    self.TT("dve", self.BBLK[:, pair:pair + 1], self.BBLK[:, pair:pair + 1], pb[:, 255:256], ALU.add,
                            R=[pb, self.BBLK], W=[self.BBLK])
                    LVL = int(os.environ.get("KLVL", "9"))
                    if LVL < 2:
                        continue
                    psc = self.psum()
                    self.MM(psc[:, 0:256], kt[:, :], qz[:, :, :].rearrange("p h t -> p (h t)"), R=[kt, qz], W=[psc])
                    sm = self.SM.next()
                    msk = self.cb("cum")[:, 0:128].unsqueeze(1).to_broadcast([128, 2, 128])
                    self.TT("dve", sm[:, :].rearrange("p (h t) -> p h t", h=2), psc[:, 0:256].rearrange("p (h t) -> p h t", h=2),
                            msk, ALU.mult, R=[psc, self.CB], W=[sm])
                    if LVL < 3:
                        continue
                    pkv = self.psum()
                    for hh in range(2):
                        h = pair * 2 + hh
                        self.MM(pkv[hh * 64:(hh + 1) * 64, 0:256], KD[:, h * 64:(h + 1) * 64],
                                VB[:, h, :, :].rearrange("p j c -> p (j c)"), R=[KD, VB], W=[pkv])
                    if LVL < 4:
                        continue
                    po = self.psum()
                    for hh in range(2):
                        h = pair * 2 + hh
                        self.MM(po[hh * 64:(hh + 1) * 64, 0:128], V[:, h * 64:(h + 1) * 64], sm[:, hh * 128:(hh + 1) * 128],
                                start=True, stop=False, R=[V, sm], W=[po])
                    for j in range(4):
                        if LVL < 5:
                            continue
                        csl = slice(j * 32, (j + 1) * 32)
                        self.MM(po[:, csl], scur[pair][:, :], qt[:, csl], start=False, stop=(j == 3),
                                R=[scur[pair], qt], W=[po])
                        S = self.S32[pair]
                        self.STT("dve", S[:], S[:], ed[:, j:j + 1], pkv[:, j * 64:(j + 1) * 64], ALU.mult, ALU.add,
                                 R=[S, ed, pkv], W=[S])
                        sn = self.SBF[pair].next()
                        for hh in range(2):
                            r = slice(hh * 64, (hh + 1) * 64)
                            self.CP("act", sn[r, r], S[r, :], R=[S], W=[sn])
                        scur[pair] = sn
                    self.CP("act", OT[:, pair, bsl], po[:, 0:128], R=[po], W=[OT])
            for pair in range(4):
                rs = slice(pair * 128, (pair + 1) * 128)
                P.dma("sp", io["GT"].t[rs, sl], GT[:, pair, :], reads=[GT], writes=[(io["GT"], (pair, tt))], final=True)
                P.dma("sp", io["QH"].t[rs, sl], QH[:, pair, :], reads=[QH], writes=[(io["QH"], (pair, tt))], final=True)
                P.dma("sp", io["OT"].t[rs, sl], OT[:, pair, :], reads=[OT], writes=[(io["OT"], (pair, tt))], final=True)
        for pair in range(4):
            P.dma("sp", io["U"].t[pair * 128:(pair + 1) * 128, :], self.S32[pair][:], reads=[self.S32[pair]], writes=[(io["U"], pair)], final=True)
        self.ACT(self.DOUT[:], self.BBLK[:], AF.Exp, R=[self.BBLK], W=[self.DOUT])
        P.dma("sp", io["D"].t, self.DOUT[:], reads=[self.DOUT], writes=[io["D"]], final=True)

    def _rope_tables(self):
        P = self.P
        if "pos" not in self.dram:
            self.din("pos", [1, TOK], I32)
        pos = self.dram["pos"]
        self.COS = self.sb([128, TOK], F32, "COS")
        self.SSIN = self.sb([128, TOK], F32, "SSIN")
        with self.scope():
            HN = TOK // 2
            pi = self.sb([128, HN], I32, "posi")
            u = self.sb([128, HN], F32, "rope_u")
            ki = self.sb([128, HN], I32, "rope_ki")
            kf = self.sb([128, HN], F32, "rope_kf")
            import os
            SK = os.environ.get("KSKIP", "")
            for hf in range(1 if "h" in SK else 2):
                hs = slice(hf * HN, (hf + 1) * HN)
                P.dma("sp", pi[:], pos.t[:, hs].partition_broadcast(128), writes=[pi])
                for dst, off in ((self.SSIN, 0.5), (self.COS, 0.75)):
                    self.CP("dve", kf[:], pi[:], R=[pi], W=[kf])
                    self.TS("dve", u[:], kf[:], self.cf("invf"), off, ALU.mult, ALU.add, R=[kf, self.CF], W=[u])
                    self.CP("dve", ki[:], u[:], R=[u], W=[ki])
                    self.CP("dve", kf[:], ki[:], R=[ki], W=[kf])
                    self.TT("dve", u[:], u[:], kf[:], ALU.subtract, R=[u, kf], W=[u])
                    self.P.op("dve", lambda e: e.tensor_single_scalar(kf[:], u[:], 0.0, ALU.is_lt), [u], [kf])
                    self.TT("dve", u[:], u[:], kf[:], ALU.add, R=[u, kf], W=[u])
                    self.ACT(dst[:, hs], u[:], AF.Copy if "n" in SK else AF.Sin, R=[u, self.negpi], W=[dst], bias=self.negpi[:], scale=float(2 * np.pi * (1 - 1e-6)))
            self.TS("dve", self.SSIN[:], self.SSIN[:], self.cf("sign"), None, ALU.mult, R=[self.SSIN, self.CF], W=[self.SSIN])

    def _attn_proj(self, l, io, w_in):
        P = self.P
        import os
        SK = os.environ.get("KSKIP", "")
        if "r" in SK:
            self.COS = self.sb([128, TOK], F32, "COS")
            self.SSIN = self.sb([128, TOK], F32, "SSIN")
            self.MS("dve", self.COS[:], 1.0, W=[self.COS])
            self.MS("dve", self.SSIN[:], 0.0, W=[self.SSIN])
        else:
            self._rope_tables()
        wq = self.load_wg(w_in, l, 0)
        wk = self.load_wg(w_in, l, 1)
        wv = self.load_wg(w_in, l, 2)
        self.RT = self.ring(3, [128, 4, 512], BF16, "ropeout")
        self.VT = self.ring(3, [128, 512], BF16, "vtok")
        perm = self.cb("perm")
        for tt in range(1 if "1" in SK else NTT):
            sl = slice(tt * 512, (tt + 1) * 512)
            for w, dst in ((wq, io["QT"]), (wk, io["KT"])):
                if "q" in SK:
                    continue
                RT = self.RT.next()
                for pair in range(4):
                    ps = self._proj_fm(w, pair, tt)
                    qb = self.B512.next()
                    self.CP("act", qb[:], ps[:], R=[ps], W=[qb])
                    ps2 = self.psum()
                    self.MM(ps2[:], perm, qb[:], R=[qb, self.CB], W=[ps2])
                    t1 = self.F512.next()
                    self.TT("dve", t1[:], ps[:], self.COS[:, sl], ALU.mult, R=[ps, self.COS, qb], W=[t1])
                    t2 = self.F512.next()
                    self.TT("dve", t2[:], ps2[:], self.SSIN[:, sl], ALU.mult, R=[ps2, self.SSIN], W=[t2])
                    self.TT(os.environ.get("KADD", "dve"), RT[:, pair, :], t1[:], t2[:], ALU.add, R=[t1, t2], W=[RT])
                for pair in range(4):
                    P.dma("sp", dst.t[pair * 128:(pair + 1) * 128, sl], RT[:, pair, :], reads=[RT], writes=[(dst, (pair, tt))], final=True)
            for tbl in range(4):
                if "v" in SK:
                    continue
                tb = tt * 4 + tbl
                ps = self._proj_tm(wv, tb)
                VT = self.VT.next()
                self.CP("act", VT[:], ps[:], R=[ps], W=[VT])
                if "s" not in SK:
                    P.dma(os.environ.get("KQ", "sp"), io["V"].t[tb * 128:(tb + 1) * 128, :], VT[:], reads=[VT], writes=[(io["V"], tb)], final=True)

    def stage_B(self, l, io):
        with self.scope():
            self._hgrn_finish(l, io)
        with self.scope():
            self._attention(l, io)
        if getattr(self, "dbgM", None) is not None and l == 0:
            for dc in range(8):
                self.P.dma("sp", self.dbgM.t[dc * 128:(dc + 1) * 128, :], self.HT[:, dc, :], reads=[(self.HT, tt) for tt in range(NTT)], final=True)
        with self.scope():
            self._out_proj(l)
        if getattr(self, "dbgX", None) is not None and l == 0:
            self.store_xT(self.dbgX)
        with self.scope():
            self._moe(l)

    def _small_in(self, name, shape, dt=F32):
        if name not in self.dram:
            self.din(name, shape, dt)
        return self.dram[name]

    def _hgrn_finish(self, l, io):
        P = self.P
        hn = self.sb([128, DEPTH, 4], F32, "hnT")
        P.dma("sp", hn[:], self._small_in("hnT", [128, DEPTH, 4]).t, writes=[hn])
        Ug = self.sb([128, 4, 8, 64], F32, "Ug")
        Dg = self.sb([128, 8, 4], F32, "Dg")
        for c in range(8):
            P.dma("sp", Dg[:, c, :], io["Dg"].t[c], writes=[Dg])
            for pair in range(4):
                P.dma("sp", Ug[:, pair, c, :], io["Ug"].t[c, pair * 128:(pair + 1) * 128, :], writes=[Ug])
        SBD = []
        for pair in range(4):
            S = self.sb([128, 64], F32, f"Sst{pair}")
            T = self.sb([128, 64], F32, f"Tst{pair}")
            self.MS("dve", S[:], 0.0, W=[S])
            for c in range(8):
                self.STT("dve", T[:], S[:], Dg[:, c, pair:pair + 1], Ug[:, pair, c, :], ALU.mult, ALU.add, R=[S, Dg, Ug], W=[T])
                self.TT("dve", T[:], T[:], S[:], ALU.subtract, R=[T, S], W=[T])
                self.STT("dve", S[:], T[:], self.PC[:, c:c + 1], S[:], ALU.mult, ALU.add, R=[T, S, self.PC], W=[S])
            bd = self.sb([128, 128], BF16, f"SBD{pair}")
            self.MS("dve", bd[:], 0.0, W=[bd])
            for hh in range(2):
                r = slice(hh * 64, (hh + 1) * 64)
                self.CP("act", bd[r, r], S[r, :], R=[S], W=[bd])
            SBD.append(bd)
        QHr = self.ring(3, [128, 512], BF16, "QHr")
        OTr = self.ring(3, [128, 512], F32, "OTr")
        GTr = self.ring(5, [128, 512], BF16, "GTr")
        REC = self.ring(5, [128, 512], F32, "RECr")
        for tt in range(NTT):
            sl = slice(tt * 512, (tt + 1) * 512)
            recs, gts = [], []
            for pair in range(4):
                rs = slice(pair * 128, (pair + 1) * 128)
                qh = QHr.next()
                P.dma("sp", qh[:], io["QH"].t[rs, sl], reads=[(io["QH"], (pair, tt))], writes=[qh])
                ot = OTr.next()
                P.dma("sp", ot[:], io["OT"].t[rs, sl], reads=[(io["OT"], (pair, tt))], writes=[ot])
                gt = GTr.next()
                P.dma("sp", gt[:], io["GT"].t[rs, sl], reads=[(io["GT"], (pair, tt))], writes=[gt])
                ps = self.psum()
                self.MM(ps[:], SBD[pair][:], qh[:], R=[SBD[pair], qh], W=[ps])
                rec = REC.next()
                self.TT("dve", rec[:], ps[:], ot[:], ALU.add, R=[ps, ot], W=[rec])
                recs.append(rec)
                gts.append(gt)
            rstd = self.rms_stats(lambda ch: recs[ch][:], 4, 512, recs)
            for pair in range(4):
                tmp = self.F512.next()
                self.STT("dve", tmp[:], recs[pair][:], hn[:, l, pair:pair + 1], rstd[:], ALU.mult, ALU.mult,
                         R=[recs[pair], hn, rstd], W=[tmp])
                self.TT("dve", self.HT[:, 4 + pair, sl], tmp[:], gts[pair][:], ALU.mult, R=[tmp, gts[pair]], W=[(self.HT, tt)])

    def _attention(self, l, io):
        P = self.P
        an = self.sb([128, DEPTH, 4], F32, "anT")
        P.dma("sp", an[:], self._small_in("anT", [128, DEPTH, 4]).t, writes=[an])
        MN = self.sb([128, 256], BF16, "maskN")
        MF = self.sb([128, 256], BF16, "maskF")
        self.CP("dve", MN[:], self.cf("maska"), R=[self.CF], W=[MN])
        self.CP("dve", MF[:], self.cf("maska"), R=[self.CF], W=[MF])
        self.TS("dve", MF[:, 0:128], MF[:, 0:128], self.PC[:, 8:9], None, ALU.mult, R=[MF, self.PC], W=[MF])
        KT = self.ring(2, [128, 2 * TOK], BF16, "KTt")
        QZ = [self.sb([128, TOK], BF16, f"QZ{hh}") for hh in range(2)]
        for hh in range(2):
            self.MS("dve", QZ[hh][:], 0.0, W=[QZ[hh]])
        ACC = self.sb([128, 2, TOK], F32, "ACC")
        VO = self.ring(4, [128, 2, 2, 128], BF16, "VO")
        for t in VO.tiles:
            self.MS("dve", t[:], 1.0, W=[t])
        PT = self.ring(3, [128, 512], BF16, "PT")
        for pair in range(4):
            rs = slice(pair * 128, (pair + 1) * 128)
            kt = KT.next()
            P.dma("sp", kt[:, 0:TOK], io["KTh"].t[rs, :], writes=[kt])
            P.dma("sp", kt[:, TOK:2 * TOK], io["KT"].t[rs, :], reads=[(io["KT"], (pair, tt)) for tt in range(NTT)], writes=[kt])
            for hh in range(2):
                r = slice(hh * 64, (hh + 1) * 64)
                P.dma("sp", QZ[hh][r, :], io["QT"].t[pair * 128 + hh * 64:pair * 128 + (hh + 1) * 64, :],
                      reads=[(io["QT"], (pair, tt)) for tt in range(NTT)], writes=[QZ[hh]])
            self.MS("dve", ACC[:], 0.0, W=[ACC])
            for d in (16, 4, 1):
                span = 128 * d
                for sp in range(TOK // span):
                    s0 = sp * span
                    mask = MF if sp == 0 else MN
                    for r_ in range(d):
                        vo = VO.next()
                        for kb in range(2):
                            if kb == 0 and sp == 0:
                                src = io["Vh"].t[TOK - span:TOK, rs]
                                rd = []
                            else:
                                st_ = s0 - span if kb == 0 else s0
                                src = io["V"].t[st_:st_ + span, rs]
                                rd = [(io["V"], tb) for tb in range(st_ // 128, (st_ + span) // 128)]
                            src = src.rearrange("(i d) (h c) -> d i h c", d=d, h=2)[r_]
                            P.dma("sp", vo[:, kb, :, 0:64], src, reads=rd, writes=[vo])
                        psc = self.psum()
                        for hh in range(2):
                            qv = QZ[hh][:, s0:s0 + span].rearrange("p (i d) -> p d i", d=d)[:, r_, :]
                            for kb in range(2):
                                k0 = TOK + s0 - (span if kb == 0 else 0)
                                kv = kt[:, k0:k0 + span].rearrange("p (i d) -> p d i", d=d)[:, r_, :]
                                c0 = (hh * 2 + kb) * 128
                                self.MM(psc[:, c0:c0 + 128], kv, qv, R=[kt, QZ[hh]], W=[psc])
                        pt = PT.next()
                        self.ACT(pt[:], psc[:], AF.Exp, R=[psc], W=[pt], scale=0.125)
                        mk = mask[:, :].unsqueeze(1).to_broadcast([128, 2, 256])
                        self.TT("dve", pt[:, :].rearrange("p (h x) -> p h x", h=2), pt[:, :].rearrange("p (h x) -> p h x", h=2), mk,
                                ALU.mult, R=[pt, mask], W=[pt])
                        po = self.psum()
                        for hh in range(2):
                            for kb in range(2):
                                c0 = (hh * 2 + kb) * 128
                                self.MM(po[:, hh * 128:(hh + 1) * 128], vo[:, kb, hh, :], pt[:, c0:c0 + 128],
                                        start=(kb == 0), stop=(kb == 1), R=[vo, pt], W=[po])
                        av = ACC[:, :, s0:s0 + span].rearrange("p h (i d) -> p h d i", d=d)[:, :, r_, :]
                        self.TT("dve", av, av, po[:, 0:256].rearrange("p (h q) -> p h q", h=2), ALU.add, R=[po, ACC], W=[ACC])
            for tt in range(NTT):
                sl = slice(tt * 512, (tt + 1) * 512)
                pn = self.psum()
                self.MM(pn[:], self.cf("seln0"), ACC[:, 0, sl], start=True, stop=False, R=[ACC, self.CF], W=[pn])
                self.MM(pn[:], self.cf("seln1"), ACC[:, 1, sl], start=False, stop=True, R=[ACC, self.CF], W=[pn])
                pd = self.psum()
                self.MM(pd[:], self.cf("seld0"), ACC[:, 0, sl], start=True, stop=False, R=[ACC, self.CF], W=[pd])
                self.MM(pd[:], self.cf("seld1"), ACC[:, 1, sl], start=False, stop=True, R=[ACC, self.CF], W=[pd])
                rd_ = self.F512.next()
                self.RECIP(rd_[:], pd[:], R=[pd], W=[rd_])
                self.TT("dve", self.HT[:, pair, sl], pn[:], rd_[:], ALU.mult, R=[pn, rd_], W=[(self.HT, tt)])
        for tt in range(NTT):
            sl = slice(tt * 512, (tt + 1) * 512)
            rstd = self.rms_stats(lambda ch: self.HT[:, ch, sl], 4, 512, [(self.HT, tt)])
            for pair in range(4):
                self.STT("dve", self.HT[:, pair, sl], self.HT[:, pair, sl], an[:, l, pair:pair + 1], rstd[:], ALU.mult, ALU.mult,
                         R=[(self.HT, tt), an, rstd], W=[(self.HT, tt)])

    def _out_proj(self, l):
        P = self.P
        w_out = self._small_in("w_out", [DEPTH, D, D])
        WO = self.sb([128, 8, D], BF16, "WO")
        for ec in range(8):
            P.dma("pool", WO[:, ec, :], w_out.t[l, ec * 128:(ec + 1) * 128, :], writes=[WO])
        for tt in range(NTT):
            sl = slice(tt * 512, (tt + 1) * 512)
            for dc in range(8):
                ps = self.psum()
                for ec in range(8):
                    self.MM(ps[:], WO[:, ec, dc * 128:(dc + 1) * 128], self.HT[:, ec, sl], start=(ec == 0), stop=(ec == 7),
                            R=[WO, (self.HT, tt)], W=[ps])
                self.STT("dve", self.XT[:, dc, sl], ps[:], self.mod(l, 2, dc), self.XT[:, dc, sl], ALU.mult, ALU.add,
                         R=[ps, self.MOD, (self.XT, tt)], W=[(self.XT, tt)])

    def norm2_hook(self, l, tt, dc, tmp):
        h2f = self.H2F.next()
        self.ACT(h2f[:], tmp[:], AF.Identity, R=[tmp, self.SCP, self.MOD], W=[h2f],
                 bias=self.mod(l, 3, dc), scale=self.mod(l, 4, dc, plus1=True))
        if dc == 0:
            self.rl_ps = [self.psum() for _ in range(4)]
        for tbl in range(4):
            self.MM(self.rl_ps[tbl][:, 0:16], h2f[:, tbl * 128:(tbl + 1) * 128], self.WR[:, dc, :],
                    start=(dc == 0), stop=(dc == 7), R=[h2f, self.WR], W=[self.rl_ps[tbl]])
        if dc == 7:
            self._route(tt)

    def _route(self, tt):
        def v3(t, a, b):
            return t[:, 0:a * b].rearrange("p (a b) -> p a b", a=a)
        R = self.RT_
        L, E, PS6, T1, T2, CG, CMB = (R[k] for k in ("L", "E", "PS6", "T1", "T2", "CG", "CMB"))
        g4, m1, m2, w1, w2, gm = (R[k] for k in ("g4", "m1", "m2", "w1", "w2", "gm"))
        dv = "dve"
        for tbl in range(4):
            self.TT(dv, L[:, tbl * 16:(tbl + 1) * 16], self.rl_ps[tbl][:, 0:16], self.BRB[:, :], ALU.add,
                    R=[self.rl_ps[tbl], self.BRB], W=[L])
        self.P.op(dv, lambda e: e.tensor_reduce(gm[:, 0:4], v3(L, 4, 16), AX.X, ALU.max), [L], [gm])
        self.TT(dv, v3(L, 4, 16), v3(L, 4, 16), gm[:, 0:4].unsqueeze(2).to_broadcast([128, 4, 16]), ALU.subtract, R=[L, gm], W=[L])
        self.ACT(E[:, 0:64], L[:, 0:64], AF.Exp, R=[L], W=[E])
        e3 = v3(E, 16, 4)
        p6 = PS6[:, 0:96].rearrange("p (a b) -> p a b", a=16)
        self.TT(dv, p6[:, :, 0:3], e3[:, :, 0:3], e3[:, :, 1:4], ALU.add, R=[E], W=[PS6])
        self.TT(dv, p6[:, :, 3:5], e3[:, :, 0:2], e3[:, :, 2:4], ALU.add, R=[E], W=[PS6])
        self.TT(dv, p6[:, :, 5:6], e3[:, :, 0:1], e3[:, :, 3:4], ALU.add, R=[E], W=[PS6])
        self.P.op(dv, lambda e: e.tensor_reduce(g4[:, 0:16], p6, AX.X, ALU.max), [PS6], [g4])
        self.P.op(dv, lambda e: e.tensor_reduce(gm[:, 0:4], v3(g4, 4, 4), AX.X, ALU.max), [g4], [gm])
        self.TT(dv, v3(g4, 4, 4), v3(g4, 4, 4), gm[:, 0:4].unsqueeze(2).to_broadcast([128, 4, 4]), ALU.is_equal, R=[g4, gm], W=[g4])
        self.TT(dv, v3(T1, 16, 4), e3, g4[:, 0:16].unsqueeze(2).to_broadcast([128, 16, 4]), ALU.mult, R=[E, g4], W=[T1])
        ing = v3(T2, 4, 4)
        self.P.op(dv, lambda e: e.tensor_reduce(ing, T1[:, 0:64].rearrange("p (t g e) -> p t e g", t=4, g=4), AX.X, ALU.add), [T1], [T2])
        self.P.op(dv, lambda e: e.tensor_reduce(m1[:, 0:4], ing, AX.X, ALU.max), [T2], [m1])
        eq1 = v3(T1, 4, 4)
        self.TT(dv, eq1, ing, m1[:, 0:4].unsqueeze(2).to_broadcast([128, 4, 4]), ALU.is_equal, R=[T2, m1], W=[T1])
        ing2 = T2[:, 16:32].rearrange("p (a b) -> p a b", a=4)
        self.STT(dv, ing2, eq1, -1e30, ing, ALU.mult, ALU.add, R=[T1, T2], W=[T2])
        self.P.op(dv, lambda e: e.tensor_reduce(m2[:, 0:4], ing2, AX.X, ALU.max), [T2], [m2])
        eq2 = T1[:, 16:32].rearrange("p (a b) -> p a b", a=4)
        self.TT(dv, eq2, ing2, m2[:, 0:4].unsqueeze(2).to_broadcast([128, 4, 4]), ALU.is_equal, R=[T2, m2], W=[T1])
        self.TT(dv, w1[:, 0:4], m1[:, 0:4], m2[:, 0:4], ALU.add, R=[m1, m2], W=[w1])
        self.RECIP(w1[:, 0:4], w1[:, 0:4], R=[w1], W=[w1])
        self.TT(dv, w2[:, 0:4], m2[:, 0:4], w1[:, 0:4], ALU.mult, R=[m2, w1], W=[w2])
        self.TT(dv, w1[:, 0:4], m1[:, 0:4], w1[:, 0:4], ALU.mult, R=[m1, w1], W=[w1])
        cg = v3(CG, 4, 4)
        self.TT(dv, cg, eq1, w1[:, 0:4].unsqueeze(2).to_broadcast([128, 4, 4]), ALU.mult, R=[T1, w1], W=[CG])
        self.TT(dv, eq2, eq2, w2[:, 0:4].unsqueeze(2).to_broadcast([128, 4, 4]), ALU.mult, R=[T1, w2], W=[T1])
        self.TT(dv, cg, cg, eq2, ALU.add, R=[CG, T1], W=[CG])
        cm = CMB[:, 0:64].rearrange("p (t g e) -> p t g e", t=4, g=4)
        self.TT(dv, cm, v3(g4, 4, 4).unsqueeze(3).to_broadcast([128, 4, 4, 4]), cg.unsqueeze(2).to_broadcast([128, 4, 4, 4]),
                ALU.mult, R=[g4, CG], W=[CMB])
        pt = self.psum()
        for tbl in range(4):
            self.MM(pt[0:16, tbl * 128:(tbl + 1) * 128], CMB[:, tbl * 16:(tbl + 1) * 16], self.cf("ident"), R=[CMB, self.CF], W=[pt])
        self.CP("act", self.COMBT[:, tt * 512:(tt + 1) * 512], pt[0:16, :], R=[pt], W=[self.COMBT])

    def _moe(self, l):
        P = self.P
        wr = self._small_in("w_routerT", [128, 8, 16])
        self.WR = self.sb([128, 8, 16], F32, "WR")
        P.dma("sp", self.WR[:], wr.t, writes=[self.WR])
        self.BRB = self.sb([128, 16], F32, "BRB")
        P.dma("sp", self.BRB[:], self._small_in("b_router", [1, 16]).t.partition_broadcast(128), writes=[self.BRB])
        self.COMBT = self.sb([16, TOK], BF16, "COMBT")
        SELf = self.sb([16, 2048], F32, "SELf")
        P.dma("sp", SELf[:], self._small_in("sele", [16, 2048]).t, writes=[SELf])
        SEL = self.sb([16, 2048], BF16, "SEL")
        self.CP("dve", SEL[:], SELf[:], R=[SELf], W=[SEL])
        self.H2F = self.ring(2, [128, 512], F32, "H2F")
        self.RT_ = {k: self.sb([128, 96], F32, "rt_" + k) for k in ("L", "E", "PS6", "T1", "T2", "CG", "CMB", "g4", "m1", "m2", "w1", "w2", "gm")}
        self.norm_mod(l, 1)
        if getattr(self, "dbgC", None) is not None and l == 0:
            P.dma("sp", self.dbgC.t, self.COMBT[:], reads=[self.COMBT], final=True)
        w_gate = self._small_in("w_gate", [DEPTH, NE, D, FF])
        w_up = self._small_in("w_up", [DEPTH, NE, D, FF])
        w_down = self._small_in("w_down", [DEPTH, NE, FF, D])
        WGr = self.ring(2, [128, 8, FF], BF16, "WGe")
        WUr = self.ring(2, [128, 8, FF], BF16, "WUe")
        WDr = self.ring(2, [128, 4, D], BF16, "WDe")
        CBr = self.ring(2, [128, 512], BF16, "CBr")
        SGr = self.ring(2, [128, 512], BF16, "SGr")
        Ar = self.ring(8, [128, 512], BF16, "Ar")

        def load(e):
            g, u, dn = WGr.next(), WUr.next(), WDr.next()
            for dc in range(8):
                P.dma("pool", g[:, dc, :], w_gate.t[l, e, dc * 128:(dc + 1) * 128, :], writes=[g])
                P.dma("pool", u[:, dc, :], w_up.t[l, e, dc * 128:(dc + 1) * 128, :], writes=[u])
            for fc in range(4):
                P.dma("pool", dn[:, fc, :], w_down.t[l, e, fc * 128:(fc + 1) * 128, :], writes=[dn])
            return g, u, dn

        nxt = load(0)
        for e in range(NE):
            g, u, dn = nxt
            if e + 1 < NE:
                nxt = load(e + 1)
            for tt in range(NTT):
                sl = slice(tt * 512, (tt + 1) * 512)
                pc = self.psum()
                self.MM(pc[:], SEL[:, e * 128:(e + 1) * 128], self.COMBT[:, sl], R=[SEL, self.COMBT], W=[pc])
                cbt = CBr.next()
                self.CP("act", cbt[:], pc[:], R=[pc], W=[cbt])
                a_t = []
                for fc in range(4):
                    pg = self.psum()
                    for dc in range(8):
                        self.MM(pg[:], g[:, dc, fc * 128:(fc + 1) * 128], self.HT[:, dc, sl], start=(dc == 0), stop=(dc == 7),
                                R=[g, (self.HT, tt)], W=[pg])
                    pu = self.psum()
                    for dc in range(8):
                        self.MM(pu[:], u[:, dc, fc * 128:(fc + 1) * 128], self.HT[:, dc, sl], start=(dc == 0), stop=(dc == 7),
                                R=[u, (self.HT, tt)], W=[pu])
                    sg = SGr.next()
                    self.ACT(sg[:], pg[:], AF.Silu, R=[pg], W=[sg])
                    a = Ar.next()
                    self.TT("dve", a[:], sg[:], pu[:], ALU.mult, R=[sg, pu], W=[a])
                    self.TT("dve", a[:], a[:], cbt[:], ALU.mult, R=[a, cbt], W=[a])
                    a_t.append(a)
                for dc in range(8):
                    py = self.psum()
                    for fc in range(4):
                        self.MM(py[:], dn[:, fc, dc * 128:(dc + 1) * 128], a_t[fc][:], start=(fc == 0), stop=(fc == 3),
                                R=[dn, a_t[fc]], W=[py])
                    self.STT("dve", self.XT[:, dc, sl], py[:], self.mod(l, 5, dc), self.XT[:, dc, sl], ALU.mult, ALU.add,
                             R=[py, self.MOD, (self.XT, tt)], W=[(self.XT, tt)])

    def final_out(self, out):
        P = self.P
        with self.scope():
            fn = self.sb([128, 8], F32, "fnT")
            P.dma("sp", fn[:], self._small_in("fnT", [128, 8]).t, writes=[fn])
            FT = self.sb([128, 8, 512], F32, "FT")
            YT = self.ring(2, [128, D], F32, "YT")
            for tt in range(NTT):
                sl = slice(tt * 512, (tt + 1) * 512)
                rstd = self.rms_stats(lambda dc: self.XT[:, dc, sl], 8, D, [(self.XT, tt)])
                for dc in range(8):
                    self.STT("dve", FT[:, dc, :], self.XT[:, dc, sl], fn[:, dc:dc + 1], rstd[:], ALU.mult, ALU.mult,
                             R=[(self.XT, tt), fn, rstd], W=[FT])
                for tbl in range(4):
                    yt = YT.next()
                    for hf in range(2):
                        ps = self.psum()
                        for j in range(4):
                            dc = hf * 4 + j
                            self.MM(ps[:, j * 128:(j + 1) * 128], FT[:, dc, tbl * 128:(tbl + 1) * 128], self.cf("ident"),
                                    R=[FT, self.CF], W=[ps])
                        self.CP("act" if hf == 0 else "dve", yt[:, hf * 512:(hf + 1) * 512], ps[:], R=[ps], W=[yt])
                    tb = tt * 4 + tbl
                    P.dma("sp", out.t[tb * 128:(tb + 1) * 128, :], yt[:], reads=[yt], final=True)

    def finish(self):
        self.P.finish()
        self.st.close()
        return self.nc


A_OUT = (("KT", [512, TOK], BF16), ("V", [TOK, 512], BF16), ("QT", [512, TOK], BF16), ("OT", [512, TOK], F32),
         ("QH", [512, TOK], BF16), ("GT", [512, TOK], BF16), ("U", [512, 64], F32), ("D", [128, 4], F32))


def _a_outs(k, l):
    return {n: k.dout(f"{n}{l}", sh, dt) for n, sh, dt in A_OUT}


def _b_ins(k, l):
    io = {n: k.din(f"{n}{l}", sh, dt) for n, sh, dt in A_OUT if n not in ("U", "D")}
    io["KTh"] = k.din(f"KTh{l}", [512, TOK], BF16)
    io["Vh"] = k.din(f"Vh{l}", [TOK, 512], BF16)
    io["Ug"] = k.din(f"Ug{l}", [8, 512, 64], F32)
    io["Dg"] = k.din(f"Dg{l}", [8, 128, 4], F32)
    return io


def build_L1():
    k = K("L1")
    k.din("w_in", [DEPTH, D, INC], F32)
    xin = k.din("x", [TOK, D], F32)
    k.setup([0])
    k.load_x_tokmajor(xin)
    k.stage_A(0, _a_outs(k, 0))
    return k.finish()


def build_L2():
    k = K("L2")
    k.din("w_in", [DEPTH, D, INC], F32)
    xin = k.din("x", [TOK, D], F32)
    import os
    if os.environ.get("KDBG"):
        k.dbgM = k.dout("dbgM", [D, TOK], BF16)
        k.dbgX = k.dout("dbgX", [D, TOK], F32)
        k.dbgC = k.dout("dbgC", [16, TOK], BF16)
    k.setup([0, 1])
    k.load_x_tokmajor(xin)
    k.stage_B(0, _b_ins(k, 0))
    k.store_xT(k.dout("xT1", [D, TOK], F32))
    k.stage_A(1, _a_outs(k, 1))
    return k.finish()


def build_L3():
    k = K("L3")
    k.setup([1])
    k.load_xT(k.din("xT1", [D, TOK], F32))
    k.stage_B(1, _b_ins(k, 1))
    k.final_out(k.dout("out", [TOK, D], F32))
    return k.finish()


_NC_CACHE = {}


def _get_nc(name):
    if name not in _NC_CACHE:
        _NC_CACHE[name] = {"L1": build_L1, "L2": build_L2, "L3": build_L3}[name]()
    return _NC_CACHE[name]


def _exchange(res, l, cores):
    z_kt = np.zeros((512, TOK), ml_dtypes.bfloat16)
    z_v = np.zeros((TOK, 512), ml_dtypes.bfloat16)
    by = {c: r for c, r in zip(cores, res)}
    ug = np.stack([np.asarray(by[c][f"U{l}"]) if c in by else np.zeros((512, 64), np.float32) for c in range(NCORES)])
    dg = np.stack([np.asarray(by[c][f"D{l}"]) if c in by else np.zeros((128, 4), np.float32) for c in range(NCORES)])
    outs = []
    for c in cores:
        r = by[c]
        d = {f"{n}{l}": np.asarray(r[f"{n}{l}"]) for n in ("KT", "V", "QT", "OT", "QH", "GT")}
        if c % 4 > 0 and (c - 1) in by:
            d[f"KTh{l}"] = np.asarray(by[c - 1][f"KT{l}"])
            d[f"Vh{l}"] = np.asarray(by[c - 1][f"V{l}"])
        else:
            d[f"KTh{l}"] = z_kt
            d[f"Vh{l}"] = z_v
        d[f"Ug{l}"] = ug
        d[f"Dg{l}"] = dg
        outs.append(d)
    return outs


def _common_inputs(inp, c):
    b, j = c // 4, c % 4
    sl = slice(j * TOK, (j + 1) * TOK)
    pc = np.zeros((128, 16), np.float32)
    for i in range(NCORES):
        if i // 4 == b and i % 4 < j:
            pc[:, i] = 1.0
    pc[:, 8] = 1.0 if j > 0 else 0.0
    sele = np.zeros((16, 16, 128), np.float32)
    for e in range(16):
        sele[e, e, :] = 1.0
    f32 = np.float32
    return {
        "consts": make_consts(), "percore": pc,
        "cT": np.ascontiguousarray(inp["c"][b].reshape(8, 128).T.astype(f32)),
        "ada_w": inp["ada_w"], "ada_bT": np.ascontiguousarray(inp["ada_b"].reshape(DEPTH, 48, 128).transpose(2, 0, 1)),
        "lb_params": inp["lb_params"], "pos": np.ascontiguousarray(inp["positions"][b:b + 1, sl].astype(np.int32)),
        "w_in": inp["w_in"], "x": np.ascontiguousarray(inp["x"][b, sl]),
        "hnT": np.ascontiguousarray(inp["hgrn_norm"].reshape(DEPTH, 4, 128).transpose(2, 0, 1)),
        "anT": np.ascontiguousarray(inp["attn_norm"].reshape(DEPTH, 4, 128).transpose(2, 0, 1)),
        "fnT": np.ascontiguousarray(inp["final_norm"].reshape(8, 128).T),
        "w_routerT": np.ascontiguousarray(inp["w_router"].reshape(8, 128, 16).transpose(1, 0, 2)),
        "b_router": np.ascontiguousarray(inp["b_router"].reshape(1, 16)),
        "sele": sele.reshape(16, 2048),
        "w_out": inp["w_out"], "w_gate": inp["w_gate"], "w_up": inp["w_up"], "w_down": inp["w_down"],
    }


L_INPUTS = {
    "L1": ("consts", "percore", "cT", "ada_w", "ada_bT", "lb_params", "pos", "w_in", "x"),
    "L2": ("consts", "percore", "cT", "ada_w", "ada_bT", "lb_params", "pos", "w_in", "x", "hnT", "anT", "w_routerT", "b_router",
           "sele", "w_out", "w_gate", "w_up", "w_down"),
    "L3": ("consts", "percore", "cT", "ada_w", "ada_bT", "hnT", "anT", "fnT", "w_routerT", "b_router", "sele", "w_out",
           "w_gate", "w_up", "w_down"),
}


def run_unfused(inp, cores):
    com = {c: _common_inputs(inp, c) for c in cores}
    ids = list(range(len(cores)))
    r1 = run_bass_kernel_spmd(_get_nc("L1"), [{k: com[c][k] for k in L_INPUTS["L1"]} for c in cores], core_ids=ids).results
    ex0 = _exchange(r1, 0, cores)
    r2 = run_bass_kernel_spmd(_get_nc("L2"), [dict({k: com[c][k] for k in L_INPUTS["L2"]}, **ex0[i]) for i, c in enumerate(cores)],
                              core_ids=ids).results
    ex1 = _exchange(r2, 1, cores)
    r3 = run_bass_kernel_spmd(_get_nc("L3"), [dict({k: com[c][k] for k in L_INPUTS["L3"]}, **ex1[i], xT1=np.asarray(r2[i]["xT1"]))
                                             for i, c in enumerate(cores)], core_ids=ids).results
    return r1, r2, r3


def kernel(x, c, positions, w_in, w_out, attn_norm, hgrn_norm, lb_params, ada_w, ada_b,
           w_router, b_router, w_gate, w_up, w_down, final_norm):
    inp = dict(x=x, c=c, positions=positions, w_in=w_in, w_out=w_out, attn_norm=attn_norm, hgrn_norm=hgrn_norm,
               lb_params=lb_params, ada_w=ada_w, ada_b=ada_b, w_router=w_router, b_router=b_router, w_gate=w_gate,
               w_up=w_up, w_down=w_down, final_norm=final_norm)
    inp = {k: np.asarray(v) for k, v in inp.items()}
    cores = list(range(NCORES))
    _, _, r3 = run_unfused(inp, cores)
    out = np.zeros((2, 4 * TOK, D), np.float32)
    for c in cores:
        out[c // 4, (c % 4) * TOK:(c % 4 + 1) * TOK] = np.asarray(r3[c]["out"])
    return out


def build_A_only(stop="all"):
    k = K("A")
    k.din("w_in", [DEPTH, D, INC], F32)
    xin = k.din("x", [TOK, D], F32)
    if stop == "setup":
        k.dout("dbgs", [128, 128], F32)
    k.setup([0])
    dbg = k.dout("dbg", [D, TOK], F32)
    dbg2 = k.dout("dbg2", [D, TOK], BF16)
    if stop == "setup":
        k.P.dma("sp", dbg.t[0:128, 0:96], k.MOD[:].rearrange("p l c -> p (l c)"), reads=[k.MOD], final=True)
        return k.finish()
    k.load_x_tokmajor(xin)
    if stop == "xload":
        k.store_xT(dbg)
        return k.finish()
    if stop == "norm":
        k.dbg_norm = dbg
        k.norm_mod(0, 0)
        for dc in range(8):
            k.P.dma("sp", dbg2.t[dc * 128:(dc + 1) * 128, :], k.HT[:, dc, :], reads=[(k.HT, tt) for tt in range(NTT)], final=True)
        return k.finish()
    io = {"KT": k.dout("KT0", [512, TOK], BF16), "V": k.dout("V0", [TOK, 512], BF16), "QT": k.dout("QT0", [512, TOK], BF16),
          "OT": k.dout("OT0", [512, TOK], F32), "QH": k.dout("QH0", [512, TOK], BF16), "GT": k.dout("GT0", [512, TOK], BF16),
          "U": k.dout("U0", [512, 64], F32), "D": k.dout("D0", [128, 4], F32)}
    if stop == "wload":
        with k.scope():
            k.WG = k.ring(4, [128, 8, 512], BF16, "wg")
            import os
            if os.environ.get("KNORM"):
                k.norm_mod(0, 0)
            w = k.load_wg(k.dram["w_in"], 0, 0)
            w = k.load_wg(k.dram["w_in"], 0, 2)
            w = k.load_wg(k.dram["w_in"], 0, 1)
            for dc in range(8):
                k.P.dma("sp", dbg2.t[dc * 128:(dc + 1) * 128, 0:512], w[:, dc, :], reads=[w], final=True)
        return k.finish()
    if stop in ("hgrn", "attn"):
        k.norm_mod(0, 0)
        with k.scope():
            k.WG = k.ring(4, [128, 8, 512], BF16, "wg")
            with k.scope():
                if stop == "hgrn":
                    k._hgrn_local(0, io, k.dram["w_in"])
                else:
                    k._attn_proj(0, io, k.dram["w_in"])
        return k.finish()
    k.stage_A(0, io)
    return k.finish()
```
